# Optimizing a Trainium2 kernel written in Bass

```python
import math
import jax, jax.numpy as jnp
from jax import lax
import numpy as np

D_MODEL = 1024
BATCH = 4
SEQ = 4096
DEPTH = 2

MIX_WIDTH = D_MODEL
GDN_HEADS = 4
GDN_HEAD_DIM = 128
GDN_WIDTH = GDN_HEADS * GDN_HEAD_DIM
GDN_CONV = 4
GDN_CHUNK = 64
NSA_HEADS = 8
NSA_KV_HEADS = 2
NSA_HEAD_DIM = 64
NSA_WIDTH = NSA_HEADS * NSA_HEAD_DIM
NSA_KV_WIDTH = NSA_KV_HEADS * NSA_HEAD_DIM
CMP_LEN = 32
CMP_STRIDE = 16
CMP_HIDDEN = 2 * NSA_HEAD_DIM
SEL_BLOCK = 64
N_SELECT = 16
WINDOW = 512
Q_BLOCK = 128
ROPE_THETA = 500000.0
ROT_DIM = NSA_HEAD_DIM // 4
D_FF = 2816
N_ADA = 9
NORM_EPS = 1e-6
IN_SIZES = (3 * GDN_WIDTH, GDN_WIDTH, GDN_HEADS, GDN_HEADS,
            NSA_WIDTH, NSA_KV_WIDTH, NSA_KV_WIDTH, NSA_KV_WIDTH,
            NSA_KV_WIDTH, NSA_KV_WIDTH, NSA_KV_WIDTH, 3 * NSA_HEADS)
IN_WIDTH = sum(IN_SIZES)

kernel_name = "hybrid_gdn_nsa_macaron_adaln"


def rms_norm(x, g):
    xf = x.astype(jnp.float32)
    y = xf * lax.rsqrt(jnp.mean(xf * xf, axis=-1, keepdims=True) + NORM_EPS)
    return (y * g.astype(jnp.float32)).astype(x.dtype)


def l2_norm(x):
    xf = x.astype(jnp.float32)
    return xf * lax.rsqrt(jnp.sum(xf * xf, axis=-1, keepdims=True) + NORM_EPS)


def modulate(h, shift, scale):
    return h * (1.0 + scale[:, None, :]) + shift[:, None, :]


def swiglu(h, w_in, w_out):
    gate, up = jnp.split(h @ w_in, 2, axis=-1)
    return (jax.nn.silu(gate) * up) @ w_out


def split_cols(x, sizes):
    offs = [int(o) for o in np.cumsum(sizes)[:-1]]
    return jnp.split(x, offs, axis=-1)


def masked_softmax(s, mask):
    s = jnp.where(mask, s.astype(jnp.float32), -jnp.inf)
    m = jnp.max(s, axis=-1, keepdims=True)
    m = jnp.where(jnp.isfinite(m), m, 0.0)
    p = jnp.exp(s - m)
    return p / jnp.maximum(jnp.sum(p, axis=-1, keepdims=True), 1e-30)


def partial_rope(x, cos, sin):
    half = ROT_DIM // 2
    xf = x[..., :ROT_DIM].astype(jnp.float32)
    x1, x2 = xf[..., :half], xf[..., half:]
    rot = jnp.concatenate([x1 * cos - x2 * sin, x2 * cos + x1 * sin], axis=-1).astype(x.dtype)
    return jnp.concatenate([rot, x[..., ROT_DIM:]], axis=-1)


def causal_dwconv(x, w):
    k = w.shape[0]
    return lax.conv_general_dilated(
        x, w[:, None, :].astype(x.dtype), window_strides=(1,), padding=[(k - 1, 0)],
        dimension_numbers=("NWC", "WIO", "NWC"), feature_group_count=x.shape[-1])


def chunk_gated_delta_rule(q, k, v, g, beta):
    B, T, H, Dk = q.shape
    Dv = v.shape[-1]
    C = GDN_CHUNK
    N = T // C

    def chunks(t):
        t = t.astype(jnp.float32).reshape((B, N, C, H) + t.shape[3:])
        return jnp.moveaxis(t, (1, 3), (0, 2))

    qc, kc, vc, bc = chunks(q), chunks(k), chunks(v), chunks(beta)
    gc = jnp.cumsum(chunks(g), axis=-1)
    causal = jnp.tril(jnp.ones((C, C), dtype=bool))
    strict = jnp.tril(jnp.ones((C, C), dtype=bool), -1)
    decay = jnp.exp(jnp.where(causal, gc[..., :, None] - gc[..., None, :], -jnp.inf))
    kb = kc * bc[..., None]
    lower = jnp.where(strict, jnp.einsum("nbhcd,nbhsd->nbhcs", kb, kc) * decay, 0.0)
    rhs = jnp.concatenate([vc * bc[..., None], kb * jnp.exp(gc)[..., None]], axis=-1)
    sol = lax.linalg.triangular_solve(lower, rhs, left_side=True, lower=True, unit_diagonal=True)
    u, w = sol[..., :Dv], sol[..., Dv:]

    def step(S, xs):
        q_i, k_i, u_i, w_i, g_i, d_i = xs
        v_new = u_i - jnp.einsum("bhcd,bhdv->bhcv", w_i, S)
        attn = jnp.einsum("bhcd,bhsd->bhcs", q_i, k_i) * d_i
        o = (jnp.einsum("bhcd,bhdv->bhcv", q_i * jnp.exp(g_i)[..., None], S)
             + jnp.einsum("bhcs,bhsv->bhcv", attn, v_new))
        g_last = g_i[..., -1:]
        S = (S * jnp.exp(g_last)[..., None]
             + jnp.einsum("bhcd,bhcv->bhdv", k_i * jnp.exp(g_last - g_i)[..., None], v_new))
        return S, o

    S0 = jnp.zeros((B, H, Dk, Dv), jnp.float32)
    _, o = lax.scan(step, S0, (qc, kc, u, w, gc, decay))
    return jnp.moveaxis(o, (0, 2), (1, 3)).reshape(B, T, H, Dv)


def gated_deltanet(qkv, z, b_raw, a_raw, conv_w, a_log, dt_bias, norm_g):
    B, T, _ = qkv.shape
    qkv = jax.nn.silu(causal_dwconv(qkv, conv_w))
    q, k, v = jnp.split(qkv, 3, axis=-1)
    hd = (B, T, GDN_HEADS, GDN_HEAD_DIM)
    q = l2_norm(q.reshape(hd)) * (GDN_HEAD_DIM ** -0.5)
    k = l2_norm(k.reshape(hd))
    v = v.reshape(hd)
    beta = jax.nn.sigmoid(b_raw.astype(jnp.float32))
    g = -jnp.exp(a_log.astype(jnp.float32)) * jax.nn.softplus(a_raw.astype(jnp.float32) + dt_bias.astype(jnp.float32))
    o = chunk_gated_delta_rule(q, k, v, g, beta)
    o = rms_norm(o, norm_g) * jax.nn.silu(z.reshape(hd).astype(jnp.float32))
    return o.reshape(B, T, GDN_WIDTH).astype(qkv.dtype)


def native_sparse_attention(q, kc, vc, ks, vs, kw, vw, gate_raw, cos, sin,
                            pe_k, w1_k, w2_k, pe_v, w1_v, w2_v, out_norm_g):
    B, T, _ = q.shape
    Hq, G, dh = NSA_HEADS, NSA_KV_HEADS, NSA_HEAD_DIM
    R = Hq // G
    scale = dh ** -0.5
    q = partial_rope(q.reshape(B, T, Hq, dh), cos, sin).reshape(B, T, G, R, dh)
    heads = lambda t: t.reshape(B, T, G, dh)
    kc = partial_rope(heads(kc), cos, sin)
    ks = partial_rope(heads(ks), cos, sin)
    kw = partial_rope(heads(kw), cos, sin)
    vc, vs, vw = heads(vc), heads(vs), heads(vw)

    n_cmp = (T - CMP_LEN) // CMP_STRIDE + 1
    cmp_idx = np.arange(n_cmp)[:, None] * CMP_STRIDE + np.arange(CMP_LEN)[None, :]

    def compress(t, pe, w1, w2):
        blk = t[:, cmp_idx] + pe[:, None, :]
        blk = jnp.moveaxis(blk, 3, 2).reshape(B, n_cmp, G, CMP_LEN * dh)
        return jax.nn.silu(blk @ w1) @ w2

    k_cmp = compress(kc, pe_k, w1_k, w2_k)
    v_cmp = compress(vc, pe_v, w1_v, w2_v)
    cmp_end = jnp.asarray(cmp_idx[:, -1])

    n_blk = T // SEL_BLOCK
    n_sel = min(N_SELECT, n_blk)
    sel_start = np.arange(n_blk) * SEL_BLOCK
    overlap = jnp.asarray(((cmp_idx[:, :1] < sel_start[None, :] + SEL_BLOCK)
                           & (cmp_idx[:, -1:] >= sel_start[None, :])).astype(np.float32))
    blk_ids = jnp.arange(n_blk)
    ks_t = jnp.moveaxis(ks, 2, 1)
    vs_t = jnp.moveaxis(vs, 2, 1)
    b_ix = jnp.arange(B)[:, None, None, None]
    g_ix = jnp.arange(G)[None, :, None, None]
    kw_pad = jnp.pad(kw, ((0, 0), (WINDOW, 0), (0, 0), (0, 0)))
    vw_pad = jnp.pad(vw, ((0, 0), (WINDOW, 0), (0, 0), (0, 0)))
    gates = jax.nn.sigmoid(gate_raw.astype(jnp.float32)).reshape(B, T, G, R, 3)

    def one_block(i):
        s = i * Q_BLOCK
        t = s + jnp.arange(Q_BLOCK)
        qb = lax.dynamic_slice_in_dim(q, s, Q_BLOCK, axis=1)
        gb = lax.dynamic_slice_in_dim(gates, s, Q_BLOCK, axis=1)
        sc = jnp.einsum("bqgrd,bngd->bgrqn", qb, k_cmp) * scale
        p_cmp = masked_softmax(sc, cmp_end[None, :] <= t[:, None])
        o_cmp = jnp.einsum("bgrqn,bngd->bqgrd", p_cmp.astype(v_cmp.dtype), v_cmp)
        imp = jnp.einsum("bgrqn,nj->bgqj", p_cmp, overlap)
        cur = t[:, None] // SEL_BLOCK
        forced = (blk_ids == 0) | (blk_ids == cur) | (blk_ids == cur - 1)
        valid = blk_ids * SEL_BLOCK <= t[:, None]
        imp = jnp.where(forced, jnp.inf, jnp.where(valid, imp, -jnp.inf))
        _, top = lax.top_k(imp, n_sel)
        tok = (top[..., None] * SEL_BLOCK + jnp.arange(SEL_BLOCK)).reshape(B, G, Q_BLOCK, n_sel * SEL_BLOCK)
        k_sel = ks_t[b_ix, g_ix, tok]
        v_sel = vs_t[b_ix, g_ix, tok]
        ss = jnp.einsum("bqgrd,bgqld->bgrql", qb, k_sel) * scale
        p_sel = masked_softmax(ss, (tok <= t[:, None])[:, :, None])
        o_sel = jnp.einsum("bgrql,bgqld->bqgrd", p_sel.astype(v_sel.dtype), v_sel)
        k_win = lax.dynamic_slice_in_dim(kw_pad, s, WINDOW + Q_BLOCK, axis=1)
        v_win = lax.dynamic_slice_in_dim(vw_pad, s, WINDOW + Q_BLOCK, axis=1)
        kp = s - WINDOW + jnp.arange(WINDOW + Q_BLOCK)
        win_mask = (kp[None, :] <= t[:, None]) & (kp[None, :] > t[:, None] - WINDOW) & (kp[None, :] >= 0)
        sw = jnp.einsum("bqgrd,bkgd->bgrqk", qb, k_win) * scale
        p_win = masked_softmax(sw, win_mask)
        o_win = jnp.einsum("bgrqk,bkgd->bqgrd", p_win.astype(v_win.dtype), v_win)
        o = gb[..., 0:1] * o_cmp + gb[..., 1:2] * o_sel + gb[..., 2:3] * o_win
        o = rms_norm(o, out_norm_g)
        return o.reshape(B, Q_BLOCK, NSA_WIDTH)

    out = lax.map(one_block, jnp.arange(T // Q_BLOCK))
    return jnp.moveaxis(out, 0, 1).reshape(B, T, NSA_WIDTH).astype(q.dtype)


def token_mix(h, w_in, conv_w, a_log, dt_bias, gdn_norm_g, pe_k, w1_k, w2_k,
              pe_v, w1_v, w2_v, nsa_norm_g, w_out, cos, sin):
    (qkv, z, b_raw, a_raw, q_n, kc, vc, ks, vs, kw, vw, g_raw) = split_cols(h @ w_in, IN_SIZES)
    y_gdn = gated_deltanet(qkv, z, b_raw, a_raw, conv_w, a_log, dt_bias, gdn_norm_g)
    y_nsa = native_sparse_attention(q_n, kc, vc, ks, vs, kw, vw, g_raw, cos, sin,
                                    pe_k, w1_k, w2_k, pe_v, w1_v, w2_v, nsa_norm_g)
    return jnp.concatenate([y_gdn, y_nsa.astype(y_gdn.dtype)], axis=-1) @ w_out


def setup_inputs(seed: int = 0) -> dict:
    key = jax.random.key(seed)
    ks = jax.random.split(key, 24)
    nrm = lambda k, shape, s: jax.random.normal(k, shape, jnp.float32) * s
    x = nrm(ks[0], (BATCH, SEQ, D_MODEL), 1.0)
    c = nrm(ks[1], (BATCH, D_MODEL), 1.0)
    positions = (jax.random.randint(ks[2], (BATCH, 1), 0, SEQ, dtype=jnp.int32)
                 + jnp.arange(SEQ, dtype=jnp.int32)[None, :])
    ada_w = nrm(ks[3], (DEPTH, D_MODEL, N_ADA * D_MODEL), 0.5 * D_MODEL ** -0.5)
    ada_b = nrm(ks[4], (DEPTH, N_ADA * D_MODEL), 0.02)
    norm_g = 1.0 + nrm(ks[5], (DEPTH, 3, D_MODEL), 0.02)
    ffn_w_in = nrm(ks[6], (DEPTH, 2, D_MODEL, 2 * D_FF), D_MODEL ** -0.5)
    ffn_w_out = nrm(ks[7], (DEPTH, 2, D_FF, D_MODEL), D_FF ** -0.5)
    mix_w_in = nrm(ks[8], (DEPTH, D_MODEL, IN_WIDTH), D_MODEL ** -0.5)
    gdn_conv_w = nrm(ks[9], (DEPTH, GDN_CONV, 3 * GDN_WIDTH), GDN_CONV ** -0.5)
    gdn_a_log = jnp.log(jax.random.uniform(ks[10], (DEPTH, GDN_HEADS), jnp.float32, 1.0, 16.0))
    dt = jnp.exp(jax.random.uniform(ks[11], (DEPTH, GDN_HEADS), jnp.float32, math.log(1e-3), math.log(1e-1)))
    gdn_dt_bias = dt + jnp.log(-jnp.expm1(-dt))
    gdn_norm_g = 1.0 + nrm(ks[12], (DEPTH, GDN_HEAD_DIM), 0.02)
    cmp_pe_k = nrm(ks[13], (DEPTH, CMP_LEN, NSA_HEAD_DIM), 0.1)
    cmp_w1_k = nrm(ks[14], (DEPTH, CMP_LEN * NSA_HEAD_DIM, CMP_HIDDEN), (CMP_LEN * NSA_HEAD_DIM) ** -0.5)
    cmp_w2_k = nrm(ks[15], (DEPTH, CMP_HIDDEN, NSA_HEAD_DIM), CMP_HIDDEN ** -0.5)
    cmp_pe_v = nrm(ks[16], (DEPTH, CMP_LEN, NSA_HEAD_DIM), 0.1)
    cmp_w1_v = nrm(ks[17], (DEPTH, CMP_LEN * NSA_HEAD_DIM, CMP_HIDDEN), (CMP_LEN * NSA_HEAD_DIM) ** -0.5)
    cmp_w2_v = nrm(ks[18], (DEPTH, CMP_HIDDEN, NSA_HEAD_DIM), CMP_HIDDEN ** -0.5)
    nsa_norm_g = 1.0 + nrm(ks[19], (DEPTH, NSA_HEAD_DIM), 0.02)
    mix_w_out = nrm(ks[20], (DEPTH, MIX_WIDTH, D_MODEL), MIX_WIDTH ** -0.5)
    final_norm_g = 1.0 + nrm(ks[21], (D_MODEL,), 0.02)
    return {"x": x, "c": c, "positions": positions, "ada_w": ada_w, "ada_b": ada_b,
            "norm_g": norm_g, "ffn_w_in": ffn_w_in, "ffn_w_out": ffn_w_out,
            "mix_w_in": mix_w_in, "gdn_conv_w": gdn_conv_w, "gdn_a_log": gdn_a_log,
            "gdn_dt_bias": gdn_dt_bias, "gdn_norm_g": gdn_norm_g,
            "cmp_pe_k": cmp_pe_k, "cmp_w1_k": cmp_w1_k, "cmp_w2_k": cmp_w2_k,
            "cmp_pe_v": cmp_pe_v, "cmp_w1_v": cmp_w1_v, "cmp_w2_v": cmp_w2_v,
            "nsa_norm_g": nsa_norm_g, "mix_w_out": mix_w_out, "final_norm_g": final_norm_g}


def reference(x, c, positions, ada_w, ada_b, norm_g, ffn_w_in, ffn_w_out, mix_w_in,
              gdn_conv_w, gdn_a_log, gdn_dt_bias, gdn_norm_g, cmp_pe_k, cmp_w1_k, cmp_w2_k,
              cmp_pe_v, cmp_w1_v, cmp_w2_v, nsa_norm_g, mix_w_out, final_norm_g):
    B, T, D = x.shape
    half = ROT_DIM // 2
    inv_freq = jnp.power(ROPE_THETA, -jnp.arange(half, dtype=jnp.float32) * (2.0 / ROT_DIM))
    ang = positions.astype(jnp.float32)[..., None] * inv_freq
    cos = jnp.cos(ang)[:, :, None, :]
    sin = jnp.sin(ang)[:, :, None, :]
    cond = jax.nn.silu(c.astype(jnp.float32))
    for l in range(DEPTH):
        mod = (cond @ ada_w[l].astype(jnp.float32) + ada_b[l].astype(jnp.float32)).reshape(B, 3, 3, D).astype(x.dtype)
        h = modulate(rms_norm(x, norm_g[l, 0]), mod[:, 0, 0], mod[:, 0, 1])
        x = x + 0.5 * mod[:, 0, 2][:, None, :] * swiglu(h, ffn_w_in[l, 0], ffn_w_out[l, 0])
        h = modulate(rms_norm(x, norm_g[l, 1]), mod[:, 1, 0], mod[:, 1, 1])
        y = token_mix(h, mix_w_in[l], gdn_conv_w[l], gdn_a_log[l], gdn_dt_bias[l], gdn_norm_g[l],
                      cmp_pe_k[l], cmp_w1_k[l], cmp_w2_k[l], cmp_pe_v[l], cmp_w1_v[l], cmp_w2_v[l],
                      nsa_norm_g[l], mix_w_out[l], cos, sin)
        x = x + mod[:, 1, 2][:, None, :] * y.astype(x.dtype)
        h = modulate(rms_norm(x, norm_g[l, 2]), mod[:, 2, 0], mod[:, 2, 1])
        x = x + 0.5 * mod[:, 2, 2][:, None, :] * swiglu(h, ffn_w_in[l, 1], ffn_w_out[l, 1])
    return rms_norm(x, final_norm_g)
```

```python
import math
import numpy as np
import concourse.bass as bass
import concourse.mybir as mybir
from concourse.bass_utils import run_bass_kernel_spmd

F32 = mybir.dt.float32
BF16 = mybir.dt.bfloat16
I32 = mybir.dt.int32
AF = mybir.ActivationFunctionType
ALU = mybir.AluOpType
AX = mybir.AxisListType


class Tl:
    __slots__ = ("h", "name", "w", "r", "psum")

    def __init__(self, h, name, psum=False):
        self.h = h
        self.name = name
        self.psum = psum
        self.w = {}
        self.r = {}

    def __getitem__(self, idx):
        return self.h[idx]

    def ap(self):
        return self.h.ap() if hasattr(self.h, "ap") else self.h[:]

    def sub(self, name):
        return Tl(self.h, self.name + "." + str(name), self.psum)


class Ev:
    __slots__ = ("sem", "val", "clock")

    def __init__(self, sem, val, clock):
        self.sem = sem
        self.val = val
        self.clock = clock


class Ctx:
    NDMA = 24

    def __init__(self, nc):
        self.nc = nc
        self.eng = {"pe": nc.tensor, "act": nc.scalar, "dve": nc.vector,
                    "pool": nc.gpsimd, "sp": nc.sync}
        self.sems = {}
        self.cnt = {}
        self.clock = {}
        for e in self.eng:
            self.sems[e] = nc.alloc_semaphore("s_" + e)
            self.cnt[e] = 0
            self.clock[e] = {}
        self.dsem = [nc.alloc_semaphore("d%d" % i) for i in range(self.NDMA)]
        self.dcnt = [0] * self.NDMA
        self.dnext = 0
        self.qnext = {}
        self.nwait = 0
        self.nins = 0
        self.uid = 0

    def sb(self, shape, dt, name=None):
        self.uid += 1
        name = (name or "t") + "_%d" % self.uid
        return Tl(self.nc.alloc_sbuf_tensor(name, list(shape), dt), name)

    def ps(self, shape, dt=F32, name=None):
        self.uid += 1
        name = (name or "p") + "_%d" % self.uid
        return Tl(self.nc.alloc_psum_tensor(name, list(shape), dt), name, psum=True)

    def dram(self, name, shape, dt, kind="Internal"):
        return Tl(self.nc.dram_tensor(name, list(shape), dt, kind=kind), name)

    def _wait(self, e, ev):
        if ev is None:
            return
        ck = self.clock[e]
        key = ev.sem
        if key == e and e == "pe":
            return
        if ck.get(key, 0) >= ev.val:
            return
        semh = self.sems[key] if isinstance(key, str) else self.dsem[key]
        self.eng[e].wait_ge(semh, ev.val)
        self.nwait += 1
        for k, v in ev.clock.items():
            if ck.get(k, 0) < v:
                ck[k] = v
        if ck.get(key, 0) < ev.val:
            ck[key] = ev.val

    def _deps(self, e, reads, writes):
        for t in reads:
            for w in list(t.w.values()):
                self._wait(e, w)
            if t.psum:
                for k, r in list(t.r.items()):
                    if k != e:
                        self._wait(e, r)
        for t in writes:
            for w in list(t.w.values()):
                self._wait(e, w)
            for r in list(t.r.values()):
                self._wait(e, r)

    def _record(self, ev, reads, writes):
        for t in reads:
            t.r[ev.sem] = ev
        for t in writes:
            t.w = {ev.sem: ev}
            t.r = {}

    def op(self, e, fn, reads=(), writes=()):
        self._deps(e, reads, writes)
        ins = fn(self.eng[e])
        self.cnt[e] += 1
        ins.then_inc(self.sems[e], 1)
        ck = dict(self.clock[e])
        ev = Ev(e, self.cnt[e], ck)
        self._record(ev, reads, writes)
        self.nins += 1
        return ev

    def dma(self, q, out, in_, reads=(), writes=(), **kw):
        lo, hi = {"sp": (0, 10), "act": (10, 16), "pool": (16, 24)}[q]
        i = self.qnext.get(q, lo)
        self.qnext[q] = lo + (i + 1 - lo) % (hi - lo)
        if self.dcnt[i] > 0:
            self._wait(q, Ev(i, self.dcnt[i], {}))
        self._deps(q, reads, writes)
        ins = self.eng[q].dma_start(out=out, in_=in_, **kw)
        self.dcnt[i] += 16
        ins.then_inc(self.dsem[i], 16)
        ev = Ev(i, self.dcnt[i], dict(self.clock[q]))
        self._record(ev, reads, writes)
        self.nins += 1
        return ev

    def finish(self, outs):
        for t in outs:
            for w in list(t.w.values()):
                self._wait("sp", w)
        for i in range(self.NDMA):
            if self.dcnt[i] > 0:
                self._wait("sp", Ev(i, self.dcnt[i], {}))


D = 1024
DFF = 2816
NF = DFF // 128
NFH = NF // 2
NTOK = 2048
NT = NTOK // 128
NB = NT // 4
EPS = 1e-6


def make_ident(c, dt=BF16):
    idt = c.sb([128, 128], dt, "ident")
    c.op("pool", lambda e: e.memset(idt[:], 0.0), writes=[idt])
    c.op("pool", lambda e: e.affine_select(out=idt[:], in_=idt[:], compare_op=ALU.not_equal, fill=1.0,
                                           base=0, pattern=[[-1, 128]], channel_multiplier=1),
         reads=[idt], writes=[idt])
    return idt


def build_tok(stages):
    nc = bass.Bass("TRN2", target_bir_lowering=False)
    c = Ctx(nc)
    x_in = c.dram("x", [NTOK, D], F32, kind="ExternalInput")
    cv = c.dram("cvec", [128, 8], F32, kind="ExternalInput")
    layers = sorted(set(s[1] for s in stages if len(s) > 1))
    ada_w = {l: c.dram("ada_w%d" % l, [D, 9 * D], F32, kind="ExternalInput") for l in layers}
    ada_b = {l: c.dram("ada_b%d" % l, [1, 9 * D], F32, kind="ExternalInput") for l in layers}
    norm_g = {l: c.dram("norm_g%d" % l, [3, D], F32, kind="ExternalInput") for l in layers}
    x_out = c.dram("x_out", [NTOK, D], F32, kind="ExternalOutput")
    xin_r = [x_in.sub(t) for t in range(NT)]
    xout_r = [x_out.sub(t) for t in range(NT)]
    state = {"src": x_in, "src_r": xin_r}

    ident = make_ident(c)
    cs = c.sb([128, 8], F32, "cs")
    c.dma("sp", cs[:], cv.ap(), reads=[cv], writes=[cs])
    cs2 = c.sb([128, 8], F32, "cs2")
    c.op("act", lambda e: e.activation(out=cs2[:], in_=cs[:], func=AF.Silu), reads=[cs], writes=[cs2])
    crep = c.sb([128, 8, 128], F32, "crep")
    c.op("dve", lambda e: e.tensor_copy(out=crep[:], in_=cs2[:].unsqueeze(2).to_broadcast([128, 8, 128])),
         reads=[cs2], writes=[crep])

    wstage = c.sb([128, 8, 256], F32, "wstage")
    w_in_sb = c.sb([128, 8, DFF], BF16, "w_in")
    w_out_sb = c.sb([128, NFH, D], BF16, "w_out")
    A_rep = c.sb([128, D], F32, "A_rep")
    B_rep = c.sb([128, D], F32, "B_rep")
    G_rep = c.sb([128, D], F32, "G_rep")
    tmp_rep = c.sb([128, D], F32, "tmp_rep")
    ps_mod = [c.ps([128, 512], F32, "psmod%d" % i) for i in range(2)]

    def mod_vec(l, idx, dst):
        c.dma("act", tmp_rep[:], ada_b[l].ap()[0:1, idx * D:(idx + 1) * D].partition_broadcast(128),
              reads=[ada_b[l]], writes=[tmp_rep])
        for q in range(4):
            c.dma("sp", wstage[:], ada_w[l].ap()[:, idx * D + q * 256:idx * D + (q + 1) * 256]
                  .rearrange("(k p) n -> p k n", p=128), reads=[ada_w[l]], writes=[wstage])
            p = ps_mod[q % 2]
            for k in range(8):
                c.op("pe", lambda e: e.matmul(p[:, 0:256], lhsT=crep[:, k, :], rhs=wstage[:, k, :],
                                              start=(k == 0), stop=(k == 7)), reads=[crep, wstage], writes=[p])
            c.op("dve", lambda e: e.tensor_tensor(out=dst[:, q * 256:(q + 1) * 256], in0=p[:, 0:256],
                                                  in1=tmp_rep[:, q * 256:(q + 1) * 256], op=ALU.add),
                 reads=[p, tmp_rep], writes=[dst])

    def norm_coeffs(l, s):
        mod_vec(l, 3 * s + 1, A_rep)
        c.dma("act", tmp_rep[:], norm_g[l].ap()[s:s + 1, :].partition_broadcast(128), reads=[norm_g[l]], writes=[tmp_rep])
        c.op("dve", lambda e: e.scalar_tensor_tensor(out=A_rep[:], in0=A_rep[:], scalar=1.0, in1=tmp_rep[:],
                                                     op0=ALU.add, op1=ALU.mult), reads=[A_rep, tmp_rep], writes=[A_rep])
        mod_vec(l, 3 * s + 0, B_rep)

    ss = c.sb([128, 1], F32, "ss")
    rstd = c.sb([128, 1], F32, "rstd")
    t1 = c.sb([128, D], F32, "t1")
    h_bf = [c.sb([128, D], BF16, "h_bf%d" % i) for i in range(2)]
    ps_tr = [c.ps([128, 8, 128], BF16, "pstr%d" % i) for i in range(2)]
    hT_all = c.sb([128, 8, NTOK], BF16, "hT_all")
    hT_r = [hT_all.sub(b) for b in range(NB)]
    xblk = c.sb([128, 4, D], F32, "xblk")
    xb_r = [xblk.sub(j) for j in range(4)]

    def load_x(t, j):
        c.dma("sp" if j % 2 == 0 else "act", xblk[:, j, :], state["src"].ap()[t * 128:(t + 1) * 128, :],
              reads=[state["src_r"][t]], writes=[xb_r[j]])

    def store_x(t, j):
        c.dma("sp" if j % 2 == 0 else "act", x_out.ap()[t * 128:(t + 1) * 128, :], xblk[:, j, :],
              reads=[xb_r[j]], writes=[xout_r[t]])

    def rstd_tile(j):
        c.op("act", lambda e: e.activation(out=t1[:], in_=xblk[:, j, :], func=AF.Square, accum_out=ss[:]),
             reads=[xb_r[j]], writes=[t1, ss])
        c.op("dve", lambda e: e.tensor_scalar(out=ss[:], in0=ss[:], scalar1=1.0 / D, scalar2=EPS,
                                              op0=ALU.mult, op1=ALU.add), reads=[ss], writes=[ss])
        c.op("act", lambda e: e.activation(out=ss[:], in_=ss[:], func=AF.Sqrt), reads=[ss], writes=[ss])
        c.op("dve", lambda e: e.reciprocal(out=rstd[:], in_=ss[:]), reads=[ss], writes=[rstd])

    def norm_mod_tile(j, dst_bf):
        rstd_tile(j)
        c.op("dve", lambda e: e.scalar_tensor_tensor(out=t1[:], in0=xblk[:, j, :], scalar=rstd[:, 0:1], in1=A_rep[:],
                                                     op0=ALU.mult, op1=ALU.mult), reads=[xb_r[j], rstd, A_rep], writes=[t1])
        c.op("pool", lambda e: e.tensor_tensor(out=dst_bf[:], in0=t1[:], in1=B_rep[:], op=ALU.add),
             reads=[t1, B_rep], writes=[dst_bf])

    def transpose_tile(src_bf, t, i):
        p = ps_tr[i % 2]
        for k in range(8):
            c.op("pe", lambda e: e.transpose(p[:, k, :], src_bf[:, k * 128:(k + 1) * 128], ident[:]),
                 reads=[src_bf, ident], writes=[p])
        c.op("act", lambda e: e.copy(out=hT_all[:, :, t * 128:(t + 1) * 128], in_=p[:]), reads=[p], writes=[hT_r[t // 4]])

    def compute_hT(l, s):
        norm_coeffs(l, s)
        for blk in range(NB):
            for j in range(4):
                t = blk * 4 + j
                load_x(t, j)
                hb = h_bf[t % 2]
                norm_mod_tile(j, hb)
                transpose_tile(hb, t, t)

    ps_g = [c.ps([128, 512], F32, "psg%d" % i) for i in range(2)]
    ps_u = [c.ps([128, 512], F32, "psu%d" % i) for i in range(2)]
    ps_o = ps_mod
    sg = [c.sb([128, 512], F32, "sg%d" % i) for i in range(2)]
    actT = c.sb([128, NFH, 512], BF16, "actT")
    otmp = c.sb([128, 512], F32, "otmp")

    def load_w(dst, col0, src_ap_fn, nk, ncols, src_t):
        for k in range(nk):
            c.dma("pool", dst[:, k, col0:col0 + ncols], src_ap_fn(k), reads=[src_t], writes=[dst])

    def resid_update(po, j, hf):
        c.op("dve", lambda e: e.tensor_tensor(out=otmp[:], in0=po[:], in1=G_rep[:, hf * 512:(hf + 1) * 512],
                                              op=ALU.mult), reads=[po, G_rep], writes=[otmp])
        c.op("pool", lambda e: e.tensor_tensor(out=xblk[:, j, hf * 512:(hf + 1) * 512],
                                               in0=xblk[:, j, hf * 512:(hf + 1) * 512], in1=otmp[:], op=ALU.add),
             reads=[xb_r[j], otmp], writes=[xb_r[j]])

    def ffn_stage(l, s, f):
        w_in = c.dram("ffn_w_in%d%d" % (l, f), [D, 2 * DFF], F32, kind="ExternalInput")
        w_out = c.dram("ffn_w_out%d%d" % (l, f), [DFF, D], F32, kind="ExternalInput")
        compute_hT(l, s)
        mod_vec(l, 3 * s + 2, G_rep)
        c.op("pool", lambda e: e.tensor_scalar(out=G_rep[:], in0=G_rep[:], scalar1=0.5, scalar2=None, op0=ALU.mult),
             reads=[G_rep], writes=[G_rep])
        for half in range(2):
            f0 = half * NFH * 128
            load_w(w_in_sb, 0, lambda k: w_in.ap()[k * 128:(k + 1) * 128, f0:f0 + NFH * 128], 8, NFH * 128, w_in)
            load_w(w_in_sb, NFH * 128, lambda k: w_in.ap()[k * 128:(k + 1) * 128, DFF + f0:DFF + f0 + NFH * 128],
                   8, NFH * 128, w_in)
            load_w(w_out_sb, 0, lambda k: w_out.ap()[f0 + k * 128:f0 + (k + 1) * 128, :], NFH, D, w_out)
            for blk in range(NB):
                for j in range(4):
                    load_x(blk * 4 + j, j)
                for fch in range(NFH):
                    pg = ps_g[fch % 2]
                    pu = ps_u[fch % 2]
                    for k in range(8):
                        c.op("pe", lambda e: e.matmul(pg[:], lhsT=w_in_sb[:, k, fch * 128:(fch + 1) * 128],
                                                      rhs=hT_all[:, k, blk * 512:(blk + 1) * 512],
                                                      start=(k == 0), stop=(k == 7)), reads=[w_in_sb, hT_r[blk]], writes=[pg])
                    for k in range(8):
                        c.op("pe", lambda e: e.matmul(pu[:], lhsT=w_in_sb[:, k, (NFH + fch) * 128:(NFH + fch + 1) * 128],
                                                      rhs=hT_all[:, k, blk * 512:(blk + 1) * 512],
                                                      start=(k == 0), stop=(k == 7)), reads=[w_in_sb, hT_r[blk]], writes=[pu])
                    sgt = sg[fch % 2]
                    c.op("act", lambda e: e.activation(out=sgt[:], in_=pg[:], func=AF.Silu), reads=[pg], writes=[sgt])
                    c.op("dve", lambda e: e.tensor_tensor(out=actT[:, fch, :], in0=sgt[:], in1=pu[:], op=ALU.mult),
                         reads=[sgt, pu], writes=[actT])
                for j in range(4):
                    for hf in range(2):
                        po = ps_o[hf]
                        for fch in range(NFH):
                            c.op("pe", lambda e: e.matmul(po[:], lhsT=actT[:, fch, j * 128:(j + 1) * 128],
                                                          rhs=w_out_sb[:, fch, hf * 512:(hf + 1) * 512],
                                                          start=(fch == 0), stop=(fch == NFH - 1)),
                                 reads=[actT, w_out_sb], writes=[po])
                        resid_update(po, j, hf)
                    store_x(blk * 4 + j, j)
            state["src"] = x_out
            state["src_r"] = xout_r

    def hmix_stage(l):
        hT_out = c.dram("hT%d" % l, [D, NTOK], BF16, kind="ExternalOutput")
        compute_hT(l, 1)
        for blk in range(NB):
            c.dma("sp", hT_out.ap()[:, blk * 512:(blk + 1) * 512].rearrange("(k p) n -> p k n", p=128),
                  hT_all[:, :, blk * 512:(blk + 1) * 512], reads=[hT_r[blk]], writes=[hT_out.sub(blk)])

    def oproj_stage(l):
        yT = c.dram("yT%d" % l, [D, NTOK], BF16, kind="ExternalInput")
        wo = c.dram("mix_w_out%d" % l, [D, D], F32, kind="ExternalInput")
        load_w(w_out_sb, 0, lambda k: wo.ap()[k * 128:(k + 1) * 128, :], 8, D, wo)
        mod_vec(l, 3 * 1 + 2, G_rep)
        for blk in range(NB):
            c.dma("sp", hT_all[:, :, blk * 512:(blk + 1) * 512],
                  yT.ap()[:, blk * 512:(blk + 1) * 512].rearrange("(k p) n -> p k n", p=128),
                  reads=[yT], writes=[hT_r[blk]])
            for j in range(4):
                t = blk * 4 + j
                load_x(t, j)
                for hf in range(2):
                    po = ps_o[hf]
                    for k in range(8):
                        c.op("pe", lambda e: e.matmul(po[:], lhsT=hT_all[:, k, t * 128:(t + 1) * 128],
                                                      rhs=w_out_sb[:, k, hf * 512:(hf + 1) * 512],
                                                      start=(k == 0), stop=(k == 7)), reads=[hT_r[blk], w_out_sb], writes=[po])
                    resid_update(po, j, hf)
                store_x(t, j)
        state["src"] = x_out
        state["src_r"] = xout_r

    def final_stage():
        fg = c.dram("final_norm_g", [1, D], F32, kind="ExternalInput")
        c.dma("act", A_rep[:], fg.ap().partition_broadcast(128), reads=[fg], writes=[A_rep])
        for t in range(NT):
            j = t % 4
            load_x(t, j)
            rstd_tile(j)
            c.op("dve", lambda e: e.scalar_tensor_tensor(out=xblk[:, j, :], in0=xblk[:, j, :], scalar=rstd[:, 0:1],
                                                         in1=A_rep[:], op0=ALU.mult, op1=ALU.mult),
                 reads=[xb_r[j], rstd, A_rep], writes=[xb_r[j]])
            store_x(t, j)
        state["src"] = x_out
        state["src_r"] = xout_r

    for st in stages:
        if st[0] == "ffn":
            ffn_stage(st[1], st[2], st[3])
        elif st[0] == "hmix":
            hmix_stage(st[1])
        elif st[0] == "oproj":
            oproj_stage(st[1])
        elif st[0] == "final":
            final_stage()
    if state["src"] is x_in:
        for t in range(NT):
            load_x(t, t % 4)
            store_x(t, t % 4)
    c.finish([])
    print("tok program: ins=%d waits=%d" % (c.nins, c.nwait))
    return nc


T = 4096
NTL = T // 128
NBK = T // 512
NEG = -30000.0
STOP = [99]


class _Stop(Exception):
    pass


class H:
    def __init__(self, c):
        self.c = c

    def mm(self, o_t, o, l_t, l, r_t, r, st=True, sp=True):
        self.c.op("pe", lambda e: e.matmul(o, lhsT=l, rhs=r, start=st, stop=sp), reads=[l_t, r_t], writes=[o_t])

    def tr(self, o_t, o, i_t, i, idt):
        self.c.op("pe", lambda e: e.transpose(o, i, idt[:]), reads=[i_t, idt], writes=[o_t])

    def act(self, o_t, o, i_t, i, func, rd=(), **kw):
        self.c.op("act", lambda e: e.activation(out=o, in_=i, func=func, **kw), reads=[i_t] + list(rd), writes=[o_t])

    def tt(self, eng, o_t, o, a_t, a, b_t, b, op):
        self.c.op(eng, lambda e: e.tensor_tensor(out=o, in0=a, in1=b, op=op), reads=[a_t, b_t], writes=[o_t])

    def stt(self, eng, o_t, o, a_t, a, scalar, b_t, b, op0, op1, rd=()):
        self.c.op("dve", lambda e: e.scalar_tensor_tensor(out=o, in0=a, scalar=scalar, in1=b, op0=op0, op1=op1),
                  reads=[a_t, b_t] + list(rd), writes=[o_t])

    def ts(self, eng, o_t, o, a_t, a, s1, s2=None, op0=ALU.mult, op1=None, rd=()):
        if op1 is None:
            self.c.op(eng, lambda e: e.tensor_scalar(out=o, in0=a, scalar1=s1, scalar2=None, op0=op0),
                      reads=[a_t] + list(rd), writes=[o_t])
        else:
            self.c.op(eng, lambda e: e.tensor_scalar(out=o, in0=a, scalar1=s1, scalar2=s2, op0=op0, op1=op1),
                      reads=[a_t] + list(rd), writes=[o_t])

    def cp(self, eng, o_t, o, i_t, i):
        if eng == "act":
            self.c.op("act", lambda e: e.copy(out=o, in_=i), reads=[i_t], writes=[o_t])
        else:
            self.c.op(eng, lambda e: e.tensor_copy(out=o, in_=i), reads=[i_t], writes=[o_t])

    def memset(self, eng, t, ap, v):
        self.c.op(eng, lambda e: e.memset(ap, v), writes=[t])

    def asel(self, t, cmp, fill, cm=1, pat=-1, base=0):
        self.c.op("pool", lambda e: e.affine_select(out=t[:], in_=t[:], compare_op=cmp, fill=fill, base=base,
                                                    pattern=[[pat, t.h.shape[-1]]], channel_multiplier=cm), reads=[t], writes=[t])


def consts(c, h):
    k = {}
    for name, dt, init, cmp, fill, cm in [
        ("ident_bf", BF16, 0.0, ALU.not_equal, 1.0, 1), ("ident_f", F32, 0.0, ALU.not_equal, 1.0, 1),
        ("UT_f", F32, 1.0, ALU.is_ge, 0.0, -1),
        ("mnegL", F32, 0.0, ALU.is_gt, NEG, 1),
        ("mnegI", F32, 0.0, ALU.is_ge, NEG, -1),
    ]:
        t = c.sb([128, 128], dt, name)
        h.memset("pool", t, t[:], init)
        h.asel(t, cmp, fill, cm=cm, pat=-cm)
        k[name] = t
    for name, dt in [("ones_bf", BF16), ("ones_f", F32)]:
        t = c.sb([128, 128], dt, name)
        h.memset("pool", t, t[:], 1.0)
        k[name] = t
    return k


def gdn_part(c, h, K, hT, yT_out, inputs):
    w_gdn_d, w_ab_d, conv_d, alog_d, dtb_d, gng_d = inputs
    w_gdn = c.sb([128, 8, 2 * 512], BF16, "w_gdn")
    for k in range(8):
        c.dma("pool", w_gdn[:, k, :], w_gdn_d.ap()[k * 128:(k + 1) * 128, :], reads=[w_gdn_d], writes=[w_gdn])
    w_ab = c.sb([128, 8, 4], BF16, "w_ab")
    c.dma("pool", w_ab[:], w_ab_d.ap().rearrange("(k p) n -> p k n", p=128), reads=[w_ab_d], writes=[w_ab])
    convw = c.sb([128, 24], F32, "convw")
    c.dma("sp", convw[:], conv_d.ap(), reads=[conv_d], writes=[convw])
    alog = c.sb([128, 2], F32, "alog")
    c.dma("sp", alog[:], alog_d.ap().partition_broadcast(128), reads=[alog_d], writes=[alog])
    dtb = c.sb([128, 2], F32, "dtb")
    c.dma("sp", dtb[:], dtb_d.ap().partition_broadcast(128), reads=[dtb_d], writes=[dtb])
    gng = c.sb([128, 1], F32, "gng")
    c.dma("sp", gng[:], gng_d.ap(), reads=[gng_d], writes=[gng])
    negA = c.sb([128, 2], F32, "negA")
    h.act(negA, negA[:], alog, alog[:], AF.Exp)
    h.ts("dve", negA, negA[:], negA, negA[:], -1.0)

    ident_bf, ident_f, ones_bf, ones_f = K["ident_bf"], K["ident_f"], K["ones_bf"], K["ones_f"]
    SC = 128 ** -0.5

    pp = [c.ps([128, 512], F32, "pp%d" % i) for i in range(2)]
    psm_t = [c.ps([128, 4, 128], F32, "psm%d" % i) for i in range(2)]
    psm = [psm_t[i // 4] for i in range(8)]
    psm_ap = [psm_t[i // 4][:, i % 4, :] for i in range(8)]
    ptr_t = c.ps([128, 8, 128], BF16, "ptr")
    ptr = [ptr_t for i in range(4)]
    ptr_ap = [ptr_t[:, i, :] for i in range(4)]
    prec_t = c.ps([128, 4, 128], F32, "prec")
    prec = [prec_t for i in range(4)]
    prec_ap = [prec_t[:, i, :] for i in range(4)]
    pg = c.ps([128, 512], F32, "pg")

    S_f = [c.sb([128, 128], F32, "S_f%d" % i) for i in range(2)]
    S_b = [c.sb([128, 128], BF16, "S_b%d" % i) for i in range(2)]
    xr = [[c.sb([128, 515], F32, "xr%d%d" % (i, j)) for j in range(3)] for i in range(2)]
    for i in range(2):
        h.memset("pool", S_f[i], S_f[i][:], 0.0)
        h.memset("pool", S_b[i], S_b[i][:], 0.0)
        for j in range(3):
            h.memset("pool", xr[i][j], xr[i][j][:, 0:3], 0.0)

    acc = c.sb([128, 512], F32, "acc")
    sil = [c.sb([128, 512], F32, "sil%d" % j) for j in range(3)]
    sz = c.sb([128, 512], F32, "sz")
    sqb = c.sb([128, 512], BF16, "sqb")
    rn = c.sb([128, 512], F32, "rn")
    qn = c.sb([128, 512], F32, "qn")
    kn = c.sb([128, 512], F32, "kn")
    qn_bf = c.sb([128, 512], BF16, "qn_bf")
    kn_bf = c.sb([128, 512], BF16, "kn_bf")
    v_bf = c.sb([128, 512], BF16, "v_bf")
    kt_bf = c.sb([128, 512], BF16, "kt_bf")
    qt_bf = c.sb([128, 512], BF16, "qt_bf")
    gc_row = c.sb([128, 512], F32, "gc_row")
    egc = c.sb([128, 512], F32, "egc")
    dg = c.sb([128, 4, 128], F32, "dg")
    o_blk = c.sb([128, 512], F32, "o_blk")
    y1 = c.sb([128, 512], F32, "y1")
    y_bf = c.sb([128, 512], BF16, "y_bf")
    graw = c.sb([128, 4, 4], F32, "graw")
    g_col = c.sb([128, 4, 2], F32, "g_col")
    beta = c.sb([128, 4, 2], F32, "beta")
    gc_col = c.sb([128, 4, 2], F32, "gc_col")
    ngc_col = c.sb([128, 4, 2], F32, "ngc_col")
    egl = c.sb([128, 4, 2], F32, "egl")
    khs = c.sb([128, 4, 2], F32, "khs")
    tmpg = c.sb([128, 4, 2], F32, "tmpg")
    tmpL = c.sb([128, 128], F32, "tmpL")
    tmpI = c.sb([128, 128], F32, "tmpI")
    EL = c.sb([128, 128], F32, "EL")
    EI = c.sb([128, 128], F32, "EI")
    Lb = [c.sb([128, 128], F32, "Lb%d" % i) for i in range(2)]
    Mb = [c.sb([128, 128], F32, "Mb%d" % i) for i in range(2)]
    Pb = [c.sb([128, 128], F32, "Pb%d" % i) for i in range(2)]
    TTb = c.sb([128, 128], BF16, "TTb")
    attnT = c.sb([128, 128], BF16, "attnT")
    khat = c.sb([128, 128], BF16, "khat")
    vtok = c.sb([128, 128], BF16, "vtok")
    r0 = c.sb([128, 128], BF16, "r0")
    vnew = c.sb([128, 128], BF16, "vnew")

    if STOP[0] <= 1:
        raise _Stop()
    for blk in range(NBK):
        tok0 = blk * 512
        for i in range(4):
            for k in range(8):
                h.mm(pg, pg[:, i * 4:(i + 1) * 4], hT, hT[:, k, tok0 + i * 128:tok0 + (i + 1) * 128], w_ab, w_ab[:, k, :],
                     st=(k == 0), sp=(k == 7))
        h.cp("dve", graw, graw[:], pg, pg[:, 0:16].rearrange("p (i f) -> p i f", f=4))
        h.tt("dve", tmpg, tmpg[:], graw, graw[:, :, 0:2], dtb, dtb[:].unsqueeze(1).to_broadcast([128, 4, 2]), ALU.add)
        h.act(tmpg, tmpg[:], tmpg, tmpg[:], AF.Exp)
        h.act(tmpg, tmpg[:], tmpg, tmpg[:], AF.Ln, bias=1.0)
        h.tt("dve", g_col, g_col[:], tmpg, tmpg[:], negA, negA[:].unsqueeze(1).to_broadcast([128, 4, 2]), ALU.mult)
        h.act(beta, beta[:], graw, graw[:, :, 2:4], AF.Sigmoid)
        h.mm(pg, pg[:, 16:24], K["UT_f"], K["UT_f"][:], g_col, g_col[:].rearrange("p i f -> p (i f)"))
        h.mm(pg, pg[:, 24:32], ones_f, ones_f[:], g_col, g_col[:].rearrange("p i f -> p (i f)"))
        h.cp("dve", gc_col, gc_col[:].rearrange("p i f -> p (i f)"), pg, pg[:, 16:24])
        h.ts("dve", ngc_col, ngc_col[:].rearrange("p i f -> p (i f)"), pg, pg[:, 16:24], -1.0)
        h.act(egl, egl[:].rearrange("p i f -> p (i f)"), pg, pg[:, 24:32], AF.Exp)
        h.tt("dve", khs, khs[:].rearrange("p i f -> p (i f)"), pg, pg[:, 24:32], gc_col,
             gc_col[:].rearrange("p i f -> p (i f)"), ALU.subtract)
        h.act(khs, khs[:], khs, khs[:], AF.Exp)

        if STOP[0] <= 2:
            raise _Stop()
        for hd in range(2):
            for j in range(4):
                p = pp[j % 2]
                for k in range(8):
                    h.mm(p, p[:], w_gdn, w_gdn[:, k, hd * 512 + j * 128:hd * 512 + (j + 1) * 128],
                         hT, hT[:, k, tok0:tok0 + 512], st=(k == 0), sp=(k == 7))
                if j == 3:
                    h.act(sz, sz[:], p, p[:], AF.Silu)
                    continue
                x = xr[hd][j]
                h.cp("act", x, x[:, 3:515], p, p[:])
                cw = lambda tap: convw[:, hd * 12 + j * 4 + tap:hd * 12 + j * 4 + tap + 1]
                eng = "dve" if j != 1 else "pool"
                h.ts(eng, acc, acc[:], x, x[:, 0:512], cw(0), rd=[convw])
                for tap in range(1, 4):
                    h.stt(eng, acc, acc[:], x, x[:, tap:tap + 512], cw(tap), acc, acc[:], ALU.mult, ALU.add, rd=[convw])
                h.cp("pool", x, x[:, 0:3], x, x[:, 512:515])
                h.act(sil[j], sil[j][:], acc, acc[:], AF.Silu)
            if STOP[0] <= 3:
                raise _Stop()
            for j, dst, dst_bf, scl in [(0, qn, qn_bf, SC), (1, kn, kn_bf, 1.0)]:
                h.act(sqb, sqb[:], sil[j], sil[j][:], AF.Square)
                p = pp[j % 2]
                h.mm(p, p[:], ones_bf, ones_bf[:], sqb, sqb[:])
                h.ts("dve", rn, rn[:], p, p[:], 1e-6, op0=ALU.add)
                h.act(rn, rn[:], rn, rn[:], AF.Sqrt)
                h.c.op("dve", lambda e: e.reciprocal(out=rn[:], in_=rn[:]), reads=[rn], writes=[rn])
                h.stt("dve", dst, dst[:], sil[j], sil[j][:], scl, rn, rn[:], ALU.mult, ALU.mult)
                h.cp("pool", dst_bf, dst_bf[:], dst, dst[:])
            h.cp("pool", v_bf, v_bf[:], sil[2], sil[2][:])
            if STOP[0] <= 4:
                raise _Stop()
            for i in range(4):
                h.ts("dve", dg, dg[:, i, :], ident_f, ident_f[:], gc_col[:, i, hd:hd + 1], rd=[gc_col])
            p = pp[0]
            h.mm(p, p[:], ones_f, ones_f[:], dg, dg[:].rearrange("p i f -> p (i f)"))
            h.cp("act", gc_row, gc_row[:], p, p[:])
            h.act(egc, egc[:], p, p[:], AF.Exp)
            h.tt("dve", kt_bf, kt_bf[:], kn, kn[:], egc, egc[:], ALU.mult)
            h.tt("pool", qt_bf, qt_bf[:], qn, qn[:], egc, egc[:], ALU.mult)

            for i in range(4):
                cs = slice(i * 128, (i + 1) * 128)
                bcol = beta[:, i, hd:hd + 1]
                if STOP[0] <= 5:
                    raise _Stop()
                h.mm(psm[0], psm_ap[0], kn, kn[:, cs], kn, kn[:, cs])
                if STOP[0] <= 5 + 0.05 * 1:
                    raise _Stop()
                h.mm(psm[1], psm_ap[1], kn_bf, kn_bf[:, cs], qn_bf, qn_bf[:, cs])
                if STOP[0] <= 5 + 0.05 * 2:
                    raise _Stop()
                h.stt("pool", tmpL, tmpL[:], gc_row, gc_row[:, cs], -1.0, K["mnegL"], K["mnegL"][:], ALU.mult, ALU.add)
                if STOP[0] <= 5 + 0.05 * 3:
                    raise _Stop()
                h.act(EL, EL[:], tmpL, tmpL[:], AF.Exp, rd=[gc_col], bias=gc_col[:, i, hd:hd + 1])
                if STOP[0] <= 5 + 0.05 * 4:
                    raise _Stop()
                h.tt("pool", tmpI, tmpI[:], gc_row, gc_row[:, cs], K["mnegI"], K["mnegI"][:], ALU.add)
                if STOP[0] <= 5 + 0.05 * 5:
                    raise _Stop()
                h.act(EI, EI[:], tmpI, tmpI[:], AF.Exp, rd=[ngc_col], bias=ngc_col[:, i, hd:hd + 1])
                if STOP[0] <= 5 + 0.05 * 6:
                    raise _Stop()
                h.stt("dve", Lb[0], Lb[0][:], psm[0], psm_ap[0], bcol, EL, EL[:], ALU.mult, ALU.mult, rd=[beta])
                if STOP[0] <= 5 + 0.05 * 7:
                    raise _Stop()
                h.tt("dve", attnT, attnT[:], psm[1], psm_ap[1], EI, EI[:], ALU.mult)
                if STOP[0] <= 5 + 0.05 * 8:
                    raise _Stop()
                h.mm(psm[2], psm_ap[2], Lb[0], Lb[0][:], ident_f, ident_f[:])
                if STOP[0] <= 5 + 0.05 * 9:
                    raise _Stop()
                h.cp("act", Mb[0], Mb[0][:], psm[2], psm_ap[2])
                if STOP[0] <= 5 + 0.05 * 10:
                    raise _Stop()
                h.tt("dve", Pb[0], Pb[0][:], ident_f, ident_f[:], psm[2], psm_ap[2], ALU.subtract)
                if STOP[0] <= 5 + 0.05 * 11:
                    raise _Stop()
                if STOP[0] <= 6:
                    raise _Stop()
                for lv in range(1, 7):
                    a, b = (lv - 1) % 2, lv % 2
                    s0, s1, s2 = psm[2 + (lv % 2) * 3], psm[3 + (lv % 2) * 3], psm[4 + (lv % 2) * 3]
                    a0, a1, a2 = psm_ap[2 + (lv % 2) * 3], psm_ap[3 + (lv % 2) * 3], psm_ap[4 + (lv % 2) * 3]
                    h.mm(s0, a0, Mb[a], Mb[a][:], Lb[a], Lb[a][:])
                    if lv < 6:
                        h.mm(s1, a1, Lb[a], Lb[a][:], Mb[a], Mb[a][:])
                    h.cp("act", Lb[b], Lb[b][:], s0, a0)
                    if lv < 6:
                        h.cp("dve", Mb[b], Mb[b][:], s1, a1)
                    h.mm(s2, a2, Lb[b], Lb[b][:], Pb[a], Pb[a][:])
                    h.tt("dve", Pb[b], Pb[b][:], Pb[a], Pb[a][:], s2, a2, ALU.add)
                h.ts("dve", TTb, TTb[:], Pb[0], Pb[0][:], bcol, rd=[beta])
                if STOP[0] <= 7:
                    raise _Stop()
                h.tr(ptr[1], ptr_ap[1], kn_bf, kn_bf[:, cs], ident_bf)
                h.ts("dve", khat, khat[:], ptr[1], ptr_ap[1], khs[:, i, hd:hd + 1], rd=[khs])
                h.tr(ptr[2], ptr_ap[2], v_bf, v_bf[:, cs], ident_bf)
                h.cp("act", vtok, vtok[:], ptr[2], ptr_ap[2])
                if STOP[0] <= 8:
                    raise _Stop()
                Sf, Sb = S_f[hd], S_b[hd]
                h.mm(prec[0], prec_ap[0], kt_bf, kt_bf[:, cs], Sb, Sb[:])
                h.tt("dve", r0, r0[:], vtok, vtok[:], prec[0], prec_ap[0], ALU.subtract)
                h.mm(prec[1], prec_ap[1], TTb, TTb[:], r0, r0[:])
                h.cp("act", vnew, vnew[:], prec[1], prec_ap[1])
                h.mm(prec[2], prec_ap[2], Sb, Sb[:], qt_bf, qt_bf[:, cs], st=True, sp=False)
                h.mm(prec[2], prec_ap[2], vnew, vnew[:], attnT, attnT[:], st=False, sp=True)
                h.cp("act", o_blk, o_blk[:, cs], prec[2], prec_ap[2])
                h.mm(prec[3], prec_ap[3], khat, khat[:], vnew, vnew[:])
                h.stt("dve", Sf, Sf[:], Sf, Sf[:], egl[:, i, hd:hd + 1], prec[3], prec_ap[3], ALU.mult, ALU.add, rd=[egl])
                h.cp("act", Sb, Sb[:], Sf, Sf[:])
            if STOP[0] <= 9:
                raise _Stop()
            h.act(sqb, sqb[:], o_blk, o_blk[:], AF.Square)
            p = pp[1]
            h.mm(p, p[:], ones_bf, ones_bf[:], sqb, sqb[:])
            h.ts("dve", rn, rn[:], p, p[:], 1.0 / 128, 1e-6, op0=ALU.mult, op1=ALU.add)
            h.act(rn, rn[:], rn, rn[:], AF.Sqrt)
            h.c.op("dve", lambda e: e.reciprocal(out=rn[:], in_=rn[:]), reads=[rn], writes=[rn])
            h.tt("dve", y1, y1[:], o_blk, o_blk[:], rn, rn[:], ALU.mult)
            h.stt("dve", y_bf, y_bf[:], y1, y1[:], gng[:, 0:1], sz, sz[:], ALU.mult, ALU.mult, rd=[gng])
            c.dma("sp", yT_out.ap()[hd * 128:(hd + 1) * 128, tok0:tok0 + 512], y_bf[:], reads=[y_bf],
                  writes=[yT_out.sub("g%d_%d" % (hd, blk))])


def build_mix(do_gdn=True, do_nsa=False):
    nc = bass.Bass("TRN2", target_bir_lowering=False)
    c = Ctx(nc)
    h = H(c)
    hT_d = c.dram("hT", [1024, T], BF16, kind="ExternalInput")
    yT_out = c.dram("yT", [256, T], BF16, kind="ExternalOutput")
    K = consts(c, h)
    hT = c.sb([128, 8, T], BF16, "hT")
    hT_r = [hT.sub(k) for k in range(8)]
    for k in range(8):
        c.dma("sp" if k % 2 == 0 else "act", hT[:, k, :], hT_d.ap()[k * 128:(k + 1) * 128, :], reads=[hT_d], writes=[hT_r[k]])
    for k in range(8):
        hT.w.update(hT_r[k].w)
    if do_gdn:
        ins = (c.dram("w_gdn", [1024, 1024], F32, kind="ExternalInput"),
               c.dram("w_ab", [1024, 4], F32, kind="ExternalInput"),
               c.dram("conv_w", [128, 24], F32, kind="ExternalInput"),
               c.dram("a_log", [1, 2], F32, kind="ExternalInput"),
               c.dram("dt_bias", [1, 2], F32, kind="ExternalInput"),
               c.dram("gdn_norm_g", [128, 1], F32, kind="ExternalInput"))
        try:
            gdn_part(c, h, K, hT, yT_out, ins)
        except _Stop:
            pass
    c.finish([])
    print("mix program: ins=%d waits=%d" % (c.nins, c.nwait))
    return nc


SCALE = 64 ** -0.5


def build_nsa():
    nc = bass.Bass("TRN2", target_bir_lowering=False)
    c = Ctx(nc)
    h = H(c)
    D = lambda name, shape, dt=F32: c.dram(name, shape, dt, kind="ExternalInput")
    hT_d = D("hT", [1024, T], BF16)
    yT_out = c.dram("yT", [256, T], BF16, kind="ExternalOutput")
    w_q_d = D("w_q", [1024, 256]); w_qs_d = D("w_qs", [1024, 64])
    w_k3_d = D("w_k3", [1024, 192]); w_k3s_d = D("w_k3s", [1024, 48])
    w_vc_d = D("w_vc", [1024, 64]); w_tok_d = D("w_tok", [1024, 140])
    pos_d = D("pos", [1, T], I32); rc_d = D("ropec", [16, 2])
    pek_d = D("pe_kT", [64, 32]); w1k_d = D("w1_k", [2048, 128]); w2k_d = D("w2_k", [128, 64])
    pev_d = D("pe_vT", [64, 32]); w1v_d = D("w1_v", [2048, 128]); w2v_d = D("w2_v", [128, 64])
    ng_d = D("nsa_norm_g", [1, 64])
    jidx_d = D("jidx", [128, 64])
    bq_d = D("bq", [128, 128])

    K = consts(c, h)
    ident_bf = K["ident_bf"]
    hT = c.sb([128, 8, T], BF16, "hT")
    subs = [hT.sub(k) for k in range(8)]
    for k in range(8):
        c.dma("sp" if k % 2 == 0 else "act", hT[:, k, :], hT_d.ap()[k * 128:(k + 1) * 128, :], reads=[hT_d], writes=[subs[k]])
    for k in range(8):
        hT.w.update(subs[k].w)

    def ldw(name, src, ncols):
        t = c.sb([128, 8, ncols], BF16, name)
        c.dma("pool", t[:], src.ap().rearrange("(k p) n -> p k n", p=128), reads=[src], writes=[t])
        return t
    w_q = ldw("w_q", w_q_d, 256); w_qs = ldw("w_qs", w_qs_d, 64)
    w_k3 = ldw("w_k3", w_k3_d, 192); w_k3s = ldw("w_k3s", w_k3s_d, 48)
    w_vc = ldw("w_vc", w_vc_d, 64); w_tok = ldw("w_tok", w_tok_d, 140)
    w1k = c.sb([64, 32, 128], BF16, "w1k"); w1v = c.sb([64, 32, 128], BF16, "w1v")
    c.dma("pool", w1k[:], w1k_d.ap().rearrange("(l p) n -> p l n", p=64), reads=[w1k_d], writes=[w1k])
    c.dma("pool", w1v[:], w1v_d.ap().rearrange("(l p) n -> p l n", p=64), reads=[w1v_d], writes=[w1v])
    w2k = c.sb([128, 64], BF16, "w2k"); w2v = c.sb([128, 64], BF16, "w2v")
    c.dma("pool", w2k[:], w2k_d.ap(), reads=[w2k_d], writes=[w2k])
    c.dma("pool", w2v[:], w2v_d.ap(), reads=[w2v_d], writes=[w2v])
    pek = c.sb([64, 32], BF16, "pek"); pev = c.sb([64, 32], BF16, "pev")
    c.dma("pool", pek[:], pek_d.ap(), reads=[pek_d], writes=[pek])
    c.dma("pool", pev[:], pev_d.ap(), reads=[pev_d], writes=[pev])
    ng = c.sb([128, 64], F32, "ng")
    c.dma("sp", ng[:], ng_d.ap().partition_broadcast(128), reads=[ng_d], writes=[ng])
    rc = c.sb([16, 2], F32, "rc")
    c.dma("sp", rc[:], rc_d.ap(), reads=[rc_d], writes=[rc])
    PB = [c.ps([128, 512], F32, "pb%d" % i) for i in range(7)]
    PT = c.ps([128, 8, 128], BF16, "pt")

    hsub = None

    qT = c.sb([64, 4, T], BF16, "qT")
    kT3 = c.sb([64, 3, T], BF16, "kT3")
    vcT = c.sb([64, T], BF16, "vcT")
    t1 = c.sb([16, 512], F32, "t1"); t2 = c.sb([16, 512], F32, "t2")
    pos_i = c.sb([16, 512], I32, "pos_i")
    ang = c.sb([16, 512], F32, "ang")
    rr = c.sb([16, 512], F32, "rr")
    rri = c.sb([16, 512], I32, "rri")
    cos16 = c.sb([16, 512], F32, "cos16"); sin16 = c.sb([16, 512], F32, "sin16")
    TWO_PI = 2 * math.pi
    for blk in range(NBK):
        bs = slice(blk * 512, (blk + 1) * 512)
        c.dma("sp", pos_i[:], pos_d.ap()[:, bs].partition_broadcast(16), reads=[pos_d], writes=[pos_i])
        h.cp("dve", ang, ang[:], pos_i, pos_i[:])
        h.ts("dve", ang, ang[:], ang, ang[:], rc[:, 0:1], rd=[rc])
        for dst, shift in [(cos16, 0.5 * math.pi), (sin16, 0.0)]:
            h.ts("dve", rr, rr[:], ang, ang[:], shift, 1.0 / TWO_PI, op0=ALU.add, op1=ALU.mult)
            h.cp("dve", rri, rri[:], rr, rr[:])
            h.cp("dve", rr, rr[:], rri, rri[:])
            h.ts("dve", dst, dst[:], ang, ang[:], shift, op0=ALU.add)
            h.stt("dve", dst, dst[:], rr, rr[:], -TWO_PI, dst, dst[:], ALU.mult, ALU.add)
            h.ts("dve", rr, rr[:], dst, dst[:], math.pi, -TWO_PI, op0=ALU.is_gt, op1=ALU.mult)
            h.tt("dve", dst, dst[:], dst, dst[:], rr, rr[:], ALU.add)
            h.ts("dve", rr, rr[:], dst, dst[:], -math.pi, TWO_PI, op0=ALU.is_lt, op1=ALU.mult)
            h.tt("dve", dst, dst[:], dst, dst[:], rr, rr[:], ALU.add)
            h.act(dst, dst[:], dst, dst[:], AF.Sin)
        h.ts("dve", sin16, sin16[:], sin16, sin16[:], rc[:, 1:2], rd=[rc])
        jobs = [(w_q, j * 64, w_qs, j * 16, qT, qT[:, j, bs]) for j in range(4)] + \
               [(w_k3, j * 64, w_k3s, j * 16, kT3, kT3[:, j, bs]) for j in range(3)]
        for ji, (w, col0, ws, scol0, dst, d) in enumerate(jobs):
            p, p2 = PB[ji % 2], PB[2 + ji % 2]
            for k in range(8):
                h.mm(p, p[0:64, :], w, w[:, k, col0:col0 + 64], hT, hT[:, k, bs], st=(k == 0), sp=(k == 7))
            for k in range(8):
                h.mm(p2, p2[0:16, :], ws, ws[:, k, scol0:scol0 + 16], hT, hT[:, k, bs], st=(k == 0), sp=(k == 7))
            h.cp("act", dst, d, p, p[0:64, :])
            h.tt("dve", t1, t1[:], p, p[0:16, :], cos16, cos16[:], ALU.mult)
            h.tt("dve", t2, t2[:], p2, p2[0:16, :], sin16, sin16[:], ALU.mult)
            h.tt("pool", dst, d[0:16, :], t1, t1[:], t2, t2[:], ALU.add)
        p = PB[4]
        for k in range(8):
            h.mm(p, p[0:64, :], w_vc, w_vc[:, k, :], hT, hT[:, k, bs], st=(k == 0), sp=(k == 7))
        h.cp("act", vcT, vcT[:, bs], p, p[0:64, :])

    vs_aug = c.sb([128, NTL, 65], BF16, "vs_aug"); vw_aug = c.sb([128, NTL, 65], BF16, "vw_aug")
    gates = c.sb([128, NTL, 12], F32, "gates")
    h.memset("pool", vs_aug, vs_aug[:, :, 64:65], 1.0)
    h.memset("pool", vw_aug, vw_aug[:, :, 64:65], 1.0)
    for t in range(NTL):
        p = PB[t % 2]
        for k in range(8):
            h.mm(p, p[:, 0:140], hT, hT[:, k, t * 128:(t + 1) * 128], w_tok, w_tok[:, k, :], st=(k == 0), sp=(k == 7))
        h.cp("act", vs_aug, vs_aug[:, t, 0:64], p, p[:, 0:64])
        h.cp("dve", vw_aug, vw_aug[:, t, 0:64], p, p[:, 64:128])
        h.act(gates, gates[:, t, :], p, p[:, 128:140], AF.Sigmoid)

    kcmpT = c.sb([64, 256], BF16, "kcmpT")
    h.memset("pool", kcmpT, kcmpT[:], 0.0)
    vcmp_aug = c.sb([128, 2, 129], BF16, "vcmp_aug")
    h.memset("pool", vcmp_aug, vcmp_aug[:], 0.0)
    h.memset("pool", vcmp_aug, vcmp_aug[:, :, 64:65], 1.0)
    ovl = [c.sb([128, 64], BF16, "ovl%d" % nt) for nt in range(2)]
    for nt in range(2):
        h.memset("pool", ovl[nt], ovl[nt][:], 1.0)
        h.asel(ovl[nt], ALU.is_ge, 0.0, cm=1, pat=-4, base=nt * 128 + 1)
        h.asel(ovl[nt], ALU.is_ge, 0.0, cm=-1, pat=4, base=3 - nt * 128)
        h.cp("pool", vcmp_aug, vcmp_aug[:, nt, 65:129], ovl[nt], ovl[nt][:])
    hb = c.sb([128, 1], F32, "hb")
    hid = c.sb([128, 256], BF16, "hid")
    for which, srcT, w1, w2, pe in [(0, lambda: kT3[:, 0, :], w1k, w2k, pek), (1, lambda: vcT[:], w1v, w2v, pev)]:
        src_t = kT3 if which == 0 else vcT
        p, pbias = PB[0], PB[1]
        for l in range(32):
            h.mm(pbias, pbias[:, 0:1], w1, w1[:, l, :], pe, pe[:, l:l + 1], st=(l == 0), sp=(l == 31))
        h.cp("dve", hb, hb[:], pbias, pbias[:, 0:1])
        for l in range(32):
            h.mm(p, p[:, 0:255], w1, w1[:, l, :], src_t, srcT()[:, l:l + 16 * 254 + 1:16], st=(l == 0), sp=(l == 31))
        h.memset("pool", hid, hid[:], 0.0)
        h.act(hid, hid[:, 0:255], p, p[:, 0:255], AF.Silu, rd=[hb], bias=hb[:, 0:1])
        if which == 0:
            p3 = PB[2]
            h.mm(p3, p3[0:64, 0:255], w2, w2[:], hid, hid[:, 0:255])
            h.cp("act", kcmpT, kcmpT[:, 0:255], p3, p3[0:64, 0:255])
        else:
            for nt in range(2):
                n = 128 if nt == 0 else 127
                p3 = PB[2 + nt]
                h.mm(p3, p3[0:n, 0:64], hid, hid[:, nt * 128:nt * 128 + n], w2, w2[:])
                h.cp("act", vcmp_aug, vcmp_aug[0:n, nt, 0:64], p3, p3[0:n, 0:64])

    Ebig = c.sb([64, T], BF16, "Ebig")
    h.memset("pool", Ebig, Ebig[:], 1.0)
    h.asel(Ebig, ALU.is_ge, 0.0, cm=-64, pat=1, base=0)
    h.asel(Ebig, ALU.is_ge, 0.0, cm=64, pat=-1, base=63)
    causneg = c.sb([128, 128], BF16, "causneg")
    h.memset("pool", causneg, causneg[:], 0.0)
    h.asel(causneg, ALU.is_ge, NEG, cm=-1, pat=1)
    lowneg = c.sb([128, 128], BF16, "lowneg")
    h.memset("pool", lowneg, lowneg[:], 0.0)
    h.asel(lowneg, ALU.is_gt, NEG, cm=1, pat=-1)

    Pt = [c.sb([128, 512], BF16, "Pt%d" % i) for i in range(2)]
    cmask = [c.sb([128, 128], BF16, "cmask%d" % i) for i in range(2)]
    adj = c.sb([128, 64], F32, "adj")
    adj2 = c.sb([128, 64], F32, "adj2")
    bq = c.sb([128, 128], F32, "bq")
    c.dma("sp", bq[:], bq_d.ap(), reads=[bq_d], writes=[bq])
    jidx = c.sb([128, 64], F32, "jidx")
    c.dma("sp", jidx[:], jidx_d.ap(), reads=[jidx_d], writes=[jidx])
    imp = c.sb([128, 64], F32, "imp")
    val2 = c.sb([128, 64], F32, "val2")
    m1 = c.sb([128, 8], F32, "m1"); m2 = c.sb([128, 8], F32, "m2")
    negsel = c.sb([128, 64], BF16, "negsel")
    negselT = c.sb([64, 128], BF16, "negselT")
    den = c.sb([128, 3, 4], F32, "den")
    wgt = c.sb([128, 3, 4], F32, "wgt")
    o = c.sb([128, 4, 64], F32, "o")
    sqj = c.sb([128, 64], F32, "sqj")
    ss = c.sb([128, 4], F32, "ss")
    rno = c.sb([128, 4], F32, "rno")
    y_bf = c.sb([128, 256], BF16, "y_bf")
    yT_sb = c.sb([128, 2, 128], BF16, "yT_sb")
    ST = [PB[0], PB[1]]
    ACC_C, ACC_I, ACC_S, ACC_W = PB[2], PB[3], PB[4], PB[5]
    step = [0]

    def attn_step(kT_t, kT_ap, qi, masks, vaug_t, vaug_ap, acc, first, last, extra=None):
        st_ = ST[step[0] % 2]
        pt_ = Pt[step[0] % 2]
        step[0] += 1
        nmm = 1 + len(masks)
        h.mm(st_, st_[:], kT_t, kT_ap, qT, qi, st=True, sp=(nmm == 1))
        for mi, (l_t, l_ap, r_t, r_ap) in enumerate(masks):
            h.mm(st_, st_[:], l_t, l_ap, r_t, r_ap, st=False, sp=(mi == len(masks) - 1))
        h.act(pt_, pt_[:], st_, st_[:], AF.Exp, scale=SCALE)
        for hh in range(4):
            h.mm(acc, acc[:, hh * 65:(hh + 1) * 65], pt_, pt_[:, hh * 128:(hh + 1) * 128], vaug_t, vaug_ap,
                 st=(first and hh == 0), sp=(last and hh == 3))
        if extra is not None:
            e_t, e_ap = extra
            for hh in range(4):
                h.mm(ACC_I, ACC_I[:, hh * 64:(hh + 1) * 64], pt_, pt_[:, hh * 128:(hh + 1) * 128], e_t, e_ap,
                     st=(first and hh == 0), sp=(last and hh == 3))

    def bc4(ap):
        return ap.unsqueeze(1).to_broadcast([ap.shape[0], 4, 128])

    for i in range(NTL):
        qi = qT[:, :, i * 128:(i + 1) * 128]
        nts = [0] if i < 16 else [0, 1]
        for ni, nt in enumerate(nts):
            cm = cmask[nt]
            h.ts("dve", cm, cm[:], bq, bq[:], float(-(128 * i - 2048 * nt - 31)), NEG, op0=ALU.is_lt, op1=ALU.mult)
            attn_step(kcmpT, kcmpT[:, nt * 128:(nt + 1) * 128], qi, [(ident_bf, ident_bf[:], cm, bc4(cm[:]))],
                      vcmp_aug, vcmp_aug[:, nt, 0:65], ACC_C, ni == 0, ni == len(nts) - 1,
                      extra=(vcmp_aug, vcmp_aug[:, nt, 65:129]))
        h.ts("dve", den, den[:, 0, :], ACC_C, ACC_C[:, 0:260].rearrange("p (h f) -> p h f", f=65)[:, :, 64], 1e-30, op0=ALU.max)
        h.c.op("dve", lambda e: e.reciprocal(out=den[:, 0, :], in_=den[:, 0, :]), reads=[den], writes=[den])
        h.ts("dve", imp, imp[:], ACC_I, ACC_I[:, 0:64], den[:, 0, 0:1], rd=[den])
        for hh in range(1, 4):
            h.stt("dve", imp, imp[:], ACC_I, ACC_I[:, hh * 64:(hh + 1) * 64], den[:, 0, hh:hh + 1], imp, imp[:],
                  ALU.mult, ALU.add, rd=[den])
        h.ts("dve", adj, adj[:], jidx, jidx[:], float(2 * i - 1), 1e9, op0=ALU.is_ge, op1=ALU.mult)
        h.ts("dve", adj2, adj2[:], jidx, jidx[:], float(2 * i), 1e9, op0=ALU.is_ge, op1=ALU.mult)
        h.tt("pool", adj, adj[:], adj, adj[:], adj2, adj2[:], ALU.add)
        h.ts("dve", adj2, adj2[:], jidx, jidx[:], float(2 * i + 1), -1e12 - 2e9, op0=ALU.is_ge, op1=ALU.mult)
        h.tt("pool", adj, adj[:], adj, adj[:], adj2, adj2[:], ALU.add)
        h.memset("pool", adj, adj[:, 0:1], 3e9)
        h.tt("dve", imp, imp[:], imp, imp[:], adj, adj[:], ALU.add)
        c.op("dve", lambda e: e.max(out=m1[:], in_=imp[:]), reads=[imp], writes=[m1])
        c.op("dve", lambda e: e.match_replace(out=val2[:], in_to_replace=m1[:], in_values=imp[:], imm_value=-3e38),
             reads=[m1, imp], writes=[val2])
        c.op("dve", lambda e: e.max(out=m2[:], in_=val2[:]), reads=[val2], writes=[m2])
        h.ts("dve", negsel, negsel[:], imp, imp[:], m2[:, 7:8], NEG, op0=ALU.is_lt, op1=ALU.mult, rd=[m2])
        h.tr(PT, PT[0:64, 0, :], negsel, negsel[:], ident_bf)
        h.cp("act", negselT, negselT[:], PT, PT[0:64, 0, :])
        for jt in range(i + 1):
            masks = [(Ebig, Ebig[:, jt * 128:(jt + 1) * 128], negselT, bc4(negselT[:]))]
            if jt == i:
                masks.append((ident_bf, ident_bf[:], causneg, bc4(causneg[:])))
            attn_step(kT3, kT3[:, 1, jt * 128:(jt + 1) * 128], qi, masks, vs_aug, vs_aug[:, jt, :], ACC_S, jt == 0, jt == i)
        j0 = max(0, i - 4)
        for jt in range(j0, i + 1):
            masks = []
            if jt == i:
                masks.append((ident_bf, ident_bf[:], causneg, bc4(causneg[:])))
            if jt == i - 4:
                masks.append((ident_bf, ident_bf[:], lowneg, bc4(lowneg[:])))
            attn_step(kT3, kT3[:, 2, jt * 128:(jt + 1) * 128], qi, masks, vw_aug, vw_aug[:, jt, :], ACC_W, jt == j0, jt == i)
        for b, acc in [(1, ACC_S), (2, ACC_W)]:
            h.ts("dve", den, den[:, b, :], acc, acc[:, 0:260].rearrange("p (h f) -> p h f", f=65)[:, :, 64], 1e-30, op0=ALU.max)
            h.c.op("dve", lambda e: e.reciprocal(out=den[:, b, :], in_=den[:, b, :]), reads=[den], writes=[den])
        h.tt("dve", wgt, wgt[:], den, den[:], gates, gates[:, i, :].rearrange("p (h b) -> p b h", b=3), ALU.mult)
        for hh in range(4):
            h.ts("dve", o, o[:, hh, :], ACC_C, ACC_C[:, hh * 65:hh * 65 + 64], wgt[:, 0, hh:hh + 1], rd=[wgt])
            h.stt("dve", o, o[:, hh, :], ACC_S, ACC_S[:, hh * 65:hh * 65 + 64], wgt[:, 1, hh:hh + 1], o, o[:, hh, :],
                  ALU.mult, ALU.add, rd=[wgt])
            h.stt("dve", o, o[:, hh, :], ACC_W, ACC_W[:, hh * 65:hh * 65 + 64], wgt[:, 2, hh:hh + 1], o, o[:, hh, :],
                  ALU.mult, ALU.add, rd=[wgt])
            h.act(sqj, sqj[:], o, o[:, hh, :], AF.Square, accum_out=ss[:, hh:hh + 1])
        h.ts("dve", rno, rno[:], ss, ss[:], 1.0 / 64, 1e-6, op0=ALU.mult, op1=ALU.add, rd=[sqj])
        h.act(rno, rno[:], rno, rno[:], AF.Sqrt)
        h.c.op("dve", lambda e: e.reciprocal(out=rno[:], in_=rno[:]), reads=[rno], writes=[rno])
        for hh in range(4):
            h.stt("dve", y_bf, y_bf[:, hh * 64:(hh + 1) * 64], o, o[:, hh, :], rno[:, hh:hh + 1], ng, ng[:],
                  ALU.mult, ALU.mult, rd=[rno])
        for hf in range(2):
            h.tr(PT, PT[:, 1 + hf, :], y_bf, y_bf[:, hf * 128:(hf + 1) * 128], ident_bf)
        h.cp("act", yT_sb, yT_sb[:], PT, PT[:, 1:3, :])
        c.dma("sp", yT_out.ap()[:, i * 128:(i + 1) * 128].rearrange("(f p) n -> p f n", p=128), yT_sb[:],
              reads=[yT_sb], writes=[yT_out.sub(i)])
    c.finish([])
    print("nsa program: ins=%d waits=%d" % (c.nins, c.nwait))
    return nc


OFF = dict(q=0, k=512, v=1024, z=1536, b=2048, a=2052, qn=2056, kc=2568, vc=2696, ks=2824, vs=2952, kw=3080, vw=3208, g=3336)

def gdn_inputs(inp, l, r, hT):
    W = inp["mix_w_in"][l]
    hs = [2 * r, 2 * r + 1]
    cols = []
    for h in hs:
        for nm in ("q", "k", "v", "z"):
            cols.append(W[:, OFF[nm] + h * 128: OFF[nm] + (h + 1) * 128])
    w_gdn = np.ascontiguousarray(np.concatenate(cols, axis=1))
    w_ab = np.ascontiguousarray(np.stack([W[:, OFF["a"] + hs[0]], W[:, OFF["a"] + hs[1]], W[:, OFF["b"] + hs[0]], W[:, OFF["b"] + hs[1]]], axis=1))
    cw = inp["gdn_conv_w"][l]
    conv = np.zeros((128, 2, 3, 4), np.float32)
    for i, h in enumerate(hs):
        for j in range(3):
            conv[:, i, j, :] = cw[:, j * 512 + h * 128: j * 512 + (h + 1) * 128].T
    return {"hT": hT, "w_gdn": w_gdn, "w_ab": w_ab, "conv_w": np.ascontiguousarray(conv.reshape(128, 24)),
            "a_log": np.ascontiguousarray(inp["gdn_a_log"][l][hs][None]), "dt_bias": np.ascontiguousarray(inp["gdn_dt_bias"][l][hs][None]),
            "gdn_norm_g": np.ascontiguousarray(inp["gdn_norm_g"][l][:, None])}


def nsa_inputs(inp, l, r, hT, b):
    import math
    W = inp["mix_w_in"][l]
    g = r
    heads = [4 * g + j for j in range(4)]
    sw = lambda c0: np.concatenate([W[:, c0 + 8:c0 + 16], W[:, c0:c0 + 8]], axis=1)
    w_q = np.concatenate([W[:, OFF["qn"] + hh * 64: OFF["qn"] + (hh + 1) * 64] for hh in heads], axis=1)
    w_qs = np.concatenate([sw(OFF["qn"] + hh * 64) for hh in heads], axis=1)
    knames = ["kc", "ks", "kw"]
    w_k3 = np.concatenate([W[:, OFF[n] + g * 64: OFF[n] + (g + 1) * 64] for n in knames], axis=1)
    w_k3s = np.concatenate([sw(OFF[n] + g * 64) for n in knames], axis=1)
    w_vc = W[:, OFF["vc"] + g * 64: OFF["vc"] + (g + 1) * 64]
    w_tok = np.concatenate([W[:, OFF["vs"] + g * 64: OFF["vs"] + (g + 1) * 64], W[:, OFF["vw"] + g * 64: OFF["vw"] + (g + 1) * 64],
                            W[:, OFF["g"] + g * 12: OFF["g"] + (g + 1) * 12]], axis=1)
    ropec = np.zeros((16, 2), np.float32)
    for p in range(16):
        ropec[p, 0] = np.float32(500000.0) ** np.float32(-(p % 8) * (2.0 / 16))
        ropec[p, 1] = -1.0 if p < 8 else 1.0
    C = np.ascontiguousarray
    jidx = (np.arange(64, dtype=np.float32)[None, :] - (np.arange(128)[:, None] >= 64).astype(np.float32)).astype(np.float32)
    bq = (np.arange(128, dtype=np.float32)[None, :] - 16.0 * np.arange(128, dtype=np.float32)[:, None]).astype(np.float32)
    return {"bq": C(bq), "jidx": C(jidx), "hT": hT, "w_q": C(w_q), "w_qs": C(w_qs), "w_k3": C(w_k3), "w_k3s": C(w_k3s), "w_vc": C(w_vc), "w_tok": C(w_tok),
            "pos": C(inp["positions"][b][None].astype(np.int32)), "ropec": ropec,
            "pe_kT": C(inp["cmp_pe_k"][l].T), "w1_k": C(inp["cmp_w1_k"][l]), "w2_k": C(inp["cmp_w2_k"][l]),
            "pe_vT": C(inp["cmp_pe_v"][l].T), "w1_v": C(inp["cmp_w1_v"][l]), "w2_v": C(inp["cmp_w2_v"][l]),
            "nsa_norm_g": C(inp["nsa_norm_g"][l][None])}


_PROGS = {}


def _prog(key, fn):
    if key not in _PROGS:
        _PROGS[key] = fn()
    return _PROGS[key]


def _run(nc, maps):
    res = run_bass_kernel_spmd(nc, maps, core_ids=list(range(8)))
    return res.results


def kernel(**inp):
    inp = {k: np.asarray(v) for k, v in inp.items()}
    C = np.ascontiguousarray
    NB_ = 4

    def tok_common(c, layers):
        b, hf = c // 2, c % 2
        m = {"cvec": C(inp["c"][b].reshape(8, 128).T.astype(np.float32))}
        for l in layers:
            m["ada_w%d" % l] = C(inp["ada_w"][l])
            m["ada_b%d" % l] = C(inp["ada_b"][l][None])
            m["norm_g%d" % l] = C(inp["norm_g"][l])
        return m

    def ffn_w(m, l, f):
        m["ffn_w_in%d%d" % (l, f)] = C(inp["ffn_w_in"][l, f])
        m["ffn_w_out%d%d" % (l, f)] = C(inp["ffn_w_out"][l, f])

    def mixer(l, hT_cores):
        hTb = [C(np.concatenate([hT_cores[2 * b], hT_cores[2 * b + 1]], axis=1)) for b in range(NB_)]
        gm = [gdn_inputs(inp, l, c % 2, hTb[c // 2]) for c in range(8)]
        rg = _run(_prog("gdn", lambda: build_mix(True, False)), gm)
        nm = [nsa_inputs(inp, l, c % 2, hTb[c // 2], c // 2) for c in range(8)]
        rn = _run(_prog("nsa", build_nsa), nm)
        yT = []
        for b in range(NB_):
            yb = np.concatenate([rg[2 * b]["yT"], rg[2 * b + 1]["yT"], rn[2 * b]["yT"], rn[2 * b + 1]["yT"]], axis=0)
            yT.append(yb)
        return [C(yT[c // 2][:, (c % 2) * 2048:(c % 2 + 1) * 2048]) for c in range(8)]

    maps = []
    for c in range(8):
        b, hf = c // 2, c % 2
        m = tok_common(c, [0])
        m["x"] = C(inp["x"][b, hf * 2048:(hf + 1) * 2048])
        ffn_w(m, 0, 0)
        maps.append(m)
    r = _run(_prog("A0", lambda: build_tok([("ffn", 0, 0, 0), ("hmix", 0)])), maps)
    xs = [r[c]["x_out"] for c in range(8)]
    yT = mixer(0, [r[c]["hT0"] for c in range(8)])
    maps = []
    for c in range(8):
        m = tok_common(c, [0, 1])
        m["x"] = xs[c]
        m["yT0"] = yT[c]
        m["mix_w_out0"] = C(inp["mix_w_out"][0])
        ffn_w(m, 0, 1)
        ffn_w(m, 1, 0)
        maps.append(m)
    r = _run(_prog("A1", lambda: build_tok([("oproj", 0), ("ffn", 0, 2, 1), ("ffn", 1, 0, 0), ("hmix", 1)])), maps)
    xs = [r[c]["x_out"] for c in range(8)]
    yT = mixer(1, [r[c]["hT1"] for c in range(8)])
    maps = []
    for c in range(8):
        m = tok_common(c, [1])
        m["x"] = xs[c]
        m["yT1"] = yT[c]
        m["mix_w_out1"] = C(inp["mix_w_out"][1])
        ffn_w(m, 1, 1)
        m["final_norm_g"] = C(inp["final_norm_g"][None])
        maps.append(m)
    r = _run(_prog("A2", lambda: build_tok([("oproj", 1), ("ffn", 1, 2, 1), ("final",)])), maps)
    out = np.zeros((4, 4096, 1024), np.float32)
    for c in range(8):
        out[c // 2, (c % 2) * 2048:(c % 2 + 1) * 2048] = r[c]["x_out"]
    return out
```
